# Optimizing a Trainium2 kernel written in Bass

```python
import jax, jax.numpy as jnp
from jax import lax
import numpy as np

D_MODEL = 1024
BATCH = 1
SEQ = 16384
DEPTH = 2

HEAD_DIM = 64
SB_HEADS = 8
MOBA_HEADS = 8
DSA_HEADS = 16
QBLK = 128
MOBA_BLOCK = 256
MOBA_TOPK = 3
DSA_TOPK = 256
IDX_HEADS = 8
IDX_DIM = 64
N_EXPERTS = 32
TOP_K = 4
D_FF = 1024
SWIGLU_LIMIT = 7.0
SWIGLU_ALPHA = 1.702
MOE_GROUP = 128
PLE_DIM = 256
ROPE_THETA = 10000.0
LN_EPS = 1e-5
DN_ALPHA = (2 * DEPTH) ** 0.25
DN_BETA = (8 * DEPTH) ** -0.25

AB_WIDTH = (SB_HEADS + MOBA_HEADS) * HEAD_DIM
AB_IN = 3 * AB_WIDTH
C_WIDTH = DSA_HEADS * HEAD_DIM
C_IN = 3 * C_WIDTH + IDX_HEADS * IDX_DIM + IDX_DIM + IDX_HEADS
N_EVEN = (DEPTH + 1) // 2
N_ODD = DEPTH // 2

kernel_name = "hybrid_stickbreak_moba_dsa_moe_deepnorm"


def layer_norm(x, g, b):
    xf = x.astype(jnp.float32)
    mu = jnp.mean(xf, axis=-1, keepdims=True)
    var = jnp.mean(jnp.square(xf - mu), axis=-1, keepdims=True)
    return ((xf - mu) * lax.rsqrt(var + LN_EPS) * g + b).astype(x.dtype)


def rope(x, pos):
    d = x.shape[-1]
    half = d // 2
    inv = ROPE_THETA ** (-jnp.arange(half, dtype=jnp.float32) / half)
    ang = pos.astype(jnp.float32)[:, None] * inv[None, :]
    shp = (1, ang.shape[0]) + (1,) * (x.ndim - 3) + (half,)
    cos = jnp.cos(ang).reshape(shp)
    sin = jnp.sin(ang).reshape(shp)
    xf = x.astype(jnp.float32)
    x1, x2 = xf[..., :half], xf[..., half:]
    return jnp.concatenate([x1 * cos - x2 * sin, x2 * cos + x1 * sin], axis=-1).astype(x.dtype)


def stick_breaking_attention(q, k, v):
    B, S, H, Dh = q.shape
    scale = Dh ** -0.5
    kpos = jnp.arange(S)

    def block(i):
        qi = lax.dynamic_slice_in_dim(q, i * QBLK, QBLK, axis=1)
        z = jnp.einsum('bqhd,bshd->bhqs', qi, k).astype(jnp.float32) * scale
        qpos = i * QBLK + jnp.arange(QBLK)
        mask = (kpos[None, :] < qpos[:, None])[None, None]
        log_stay = jnp.where(mask, jax.nn.log_sigmoid(-z), 0.0)
        suffix = lax.cumsum(log_stay, axis=3, reverse=True) - log_stay
        a = jnp.where(mask, jnp.exp(jax.nn.log_sigmoid(z) + suffix), 0.0)
        return jnp.einsum('bhqs,bshd->bqhd', a.astype(v.dtype), v)

    out = lax.map(block, jnp.arange(S // QBLK))
    return out.transpose(1, 0, 2, 3, 4).reshape(B, S, H, Dh)


def moba_attention(q, k, v):
    B, S, H, Dh = q.shape
    scale = Dh ** -0.5
    nb = -(-S // MOBA_BLOCK)
    pad = nb * MOBA_BLOCK - S
    kp = jnp.pad(k, ((0, 0), (0, pad), (0, 0), (0, 0)))
    vp = jnp.pad(v, ((0, 0), (0, pad), (0, 0), (0, 0)))
    kblk = kp.reshape(B, nb, MOBA_BLOCK, H, Dh)
    vblk = vp.reshape(B, nb, MOBA_BLOCK, H, Dh)
    kmean = jnp.mean(kblk.astype(jnp.float32), axis=2).astype(k.dtype)
    kbh = kblk.transpose(0, 3, 1, 2, 4)
    vbh = vblk.transpose(0, 3, 1, 2, 4)
    n_sel = min(MOBA_TOPK, nb - 1)
    bi = jnp.arange(B)[:, None, None, None]
    hi = jnp.arange(H)[None, :, None, None]
    own_off = jnp.arange(MOBA_BLOCK)

    def block(i):
        q0 = i * QBLK
        qi = lax.dynamic_slice_in_dim(q, q0, QBLK, axis=1)
        qh = qi.transpose(0, 2, 1, 3)
        qpos = q0 + jnp.arange(QBLK)
        cur = q0 // MOBA_BLOCK
        k_own = lax.dynamic_index_in_dim(kblk, cur, axis=1, keepdims=False)
        v_own = lax.dynamic_index_in_dim(vblk, cur, axis=1, keepdims=False)
        s_own = jnp.einsum('bhqd,bkhd->bhqk', qh, k_own).astype(jnp.float32) * scale
        own_pos = cur * MOBA_BLOCK + own_off
        s_own = jnp.where((own_pos[None, :] <= qpos[:, None])[None, None], s_own, -jnp.inf)
        if n_sel == 0:
            pr = jax.nn.softmax(s_own, axis=-1)
            return jnp.einsum('bhqk,bkhd->bqhd', pr.astype(v.dtype), v_own)
        gate = jnp.einsum('bhqd,bnhd->bhqn', qh, kmean).astype(jnp.float32)
        gate = jnp.where((jnp.arange(nb) < cur)[None, None, None], gate, -jnp.inf)
        _, g_idx = lax.top_k(gate, n_sel)
        valid = g_idx < cur
        k_sel = kbh[bi, hi, g_idx]
        v_sel = vbh[bi, hi, g_idx]
        s_sel = jnp.einsum('bhqd,bhqjkd->bhqjk', qh, k_sel).astype(jnp.float32) * scale
        s_sel = jnp.where(valid[..., None], s_sel, -jnp.inf).reshape(B, H, QBLK, n_sel * MOBA_BLOCK)
        pr = jax.nn.softmax(jnp.concatenate([s_own, s_sel], axis=-1), axis=-1).astype(v.dtype)
        p_own = pr[..., :MOBA_BLOCK]
        p_sel = pr[..., MOBA_BLOCK:].reshape(B, H, QBLK, n_sel, MOBA_BLOCK)
        return (jnp.einsum('bhqk,bkhd->bqhd', p_own, v_own)
                + jnp.einsum('bhqjk,bhqjkd->bqhd', p_sel, v_sel))

    out = lax.map(block, jnp.arange(S // QBLK))
    return out.transpose(1, 0, 2, 3, 4).reshape(B, S, H, Dh)


def dsa_attention(q, k, v, qi, ki, wi):
    B, S, H, Dh = q.shape
    scale = Dh ** -0.5
    idx_scale = IDX_DIM ** -0.5
    topk = min(DSA_TOPK, S // 4)
    kpos = jnp.arange(S)
    bi = jnp.arange(B)[:, None, None]

    def block(i):
        q0 = i * QBLK
        qb = lax.dynamic_slice_in_dim(q, q0, QBLK, axis=1)
        qib = lax.dynamic_slice_in_dim(qi, q0, QBLK, axis=1)
        wib = lax.dynamic_slice_in_dim(wi, q0, QBLK, axis=1).astype(jnp.float32)
        qpos = q0 + jnp.arange(QBLK)
        logits = jnp.einsum('bqhd,bsd->bqhs', qib, ki).astype(jnp.float32) * idx_scale
        score = jnp.einsum('bqh,bqhs->bqs', wib, jax.nn.relu(logits))
        causal = (kpos[None, :] <= qpos[:, None])[None]
        score = jnp.where(causal, score, -jnp.inf)
        _, sel = lax.top_k(score, topk)
        valid = sel <= qpos[None, :, None]
        k_sel = k[bi, sel]
        v_sel = v[bi, sel]
        s = jnp.einsum('bqhd,bqkhd->bhqk', qb, k_sel).astype(jnp.float32) * scale
        s = jnp.where(valid[:, None], s, -jnp.inf)
        pr = jax.nn.softmax(s, axis=-1).astype(v.dtype)
        return jnp.einsum('bhqk,bqkhd->bqhd', pr, v_sel)

    out = lax.map(block, jnp.arange(S // QBLK))
    return out.transpose(1, 0, 2, 3, 4).reshape(B, S, H, Dh)


def mixer_ab(x, w_in, w_out):
    B, S, _ = x.shape
    pos = jnp.arange(S)
    h = x @ w_in
    qa, ka, va, qb, kb, vb = jnp.split(h, 6, axis=-1)
    shp_a = (B, S, SB_HEADS, HEAD_DIM)
    shp_b = (B, S, MOBA_HEADS, HEAD_DIM)
    oa = stick_breaking_attention(qa.reshape(shp_a), ka.reshape(shp_a), va.reshape(shp_a))
    ob = moba_attention(rope(qb.reshape(shp_b), pos), rope(kb.reshape(shp_b), pos), vb.reshape(shp_b))
    o = jnp.concatenate([oa.reshape(B, S, -1), ob.reshape(B, S, -1)], axis=-1)
    return o @ w_out


def mixer_c(x, w_in, w_out):
    B, S, _ = x.shape
    pos = jnp.arange(S)
    h = x @ w_in
    cuts = np.cumsum([C_WIDTH, C_WIDTH, C_WIDTH, IDX_HEADS * IDX_DIM, IDX_DIM]).tolist()
    q, k, v, qi, ki, wi = jnp.split(h, cuts, axis=-1)
    shp = (B, S, DSA_HEADS, HEAD_DIM)
    q = rope(q.reshape(shp), pos)
    k = rope(k.reshape(shp), pos)
    qi = rope(qi.reshape(B, S, IDX_HEADS, IDX_DIM), pos)
    ki = rope(ki, pos)
    wi = wi * IDX_HEADS ** -0.5
    o = dsa_attention(q, k, v.reshape(shp), qi, ki, wi)
    return o.reshape(B, S, -1) @ w_out


def moe(x, w_r, b_r, w1, b1, w2, b2):
    B, S, D = x.shape
    T = B * S
    x2 = x.reshape(T, D)
    logits = (x2 @ w_r + b_r).astype(jnp.float32)
    top_val, top_idx = lax.top_k(logits, TOP_K)
    gates = jax.nn.softmax(top_val, axis=-1)
    n_assign = T * TOP_K
    flat_e = top_idx.reshape(-1)
    flat_tok = jnp.arange(n_assign) // TOP_K
    order = jnp.argsort(flat_e)
    sorted_e = flat_e[order]
    sorted_tok = flat_tok[order]
    gate_sorted = gates.reshape(-1)[order].astype(x.dtype)
    counts = jnp.bincount(flat_e, length=N_EXPERTS)
    starts = jnp.cumsum(counts) - counts
    pcounts = (counts + MOE_GROUP - 1) // MOE_GROUP * MOE_GROUP
    pends = jnp.cumsum(pcounts)
    pstarts = pends - pcounts
    dest = pstarts[sorted_e] + (jnp.arange(n_assign) - starts[sorted_e])
    P = n_assign + N_EXPERTS * MOE_GROUP
    n_grp = P // MOE_GROUP
    buf = jnp.zeros((P, D), x.dtype).at[dest].set(x2[sorted_tok])
    grp_e = jnp.minimum(jnp.searchsorted(pends, jnp.arange(n_grp) * MOE_GROUP, side='right'), N_EXPERTS - 1)

    def expert_group(args):
        xb, e = args
        hgu = xb @ w1[e] + b1[e]
        g = jnp.minimum(hgu[:, 0::2], SWIGLU_LIMIT)
        u = jnp.clip(hgu[:, 1::2], -SWIGLU_LIMIT, SWIGLU_LIMIT)
        glu = g * jax.nn.sigmoid(SWIGLU_ALPHA * g)
        return ((u + 1.0) * glu) @ w2[e] + b2[e]

    out = lax.map(expert_group, (buf.reshape(n_grp, MOE_GROUP, D), grp_e)).reshape(P, D)
    y_sorted = out[dest] * gate_sorted[:, None]
    y = jax.ops.segment_sum(y_sorted, sorted_tok, num_segments=T)
    return y.reshape(B, S, D)


def setup_inputs(seed: int = 0) -> dict:
    key = jax.random.key(seed)
    ks = jax.random.split(key, 17)
    nrm = jax.random.normal
    f32 = jnp.float32
    return {
        "x": nrm(ks[0], (BATCH, SEQ, D_MODEL), f32),
        "p": nrm(ks[1], (DEPTH, BATCH, SEQ, PLE_DIM), f32),
        "ab_w_in": nrm(ks[2], (N_EVEN, D_MODEL, AB_IN), f32) * D_MODEL ** -0.5,
        "ab_w_out": nrm(ks[3], (N_EVEN, AB_WIDTH, D_MODEL), f32) * (AB_WIDTH ** -0.5 * DN_BETA),
        "c_w_in": nrm(ks[4], (N_ODD, D_MODEL, C_IN), f32) * D_MODEL ** -0.5,
        "c_w_out": nrm(ks[5], (N_ODD, C_WIDTH, D_MODEL), f32) * (C_WIDTH ** -0.5 * DN_BETA),
        "ln_g": 1.0 + 0.01 * nrm(ks[6], (DEPTH, 2, D_MODEL), f32),
        "ln_b": 0.01 * nrm(ks[7], (DEPTH, 2, D_MODEL), f32),
        "router_w": nrm(ks[8], (DEPTH, D_MODEL, N_EXPERTS), f32) * D_MODEL ** -0.5,
        "router_b": 0.01 * nrm(ks[9], (DEPTH, N_EXPERTS), f32),
        "moe_w1": nrm(ks[10], (DEPTH, N_EXPERTS, D_MODEL, 2 * D_FF), f32) * D_MODEL ** -0.5,
        "moe_b1": 0.01 * nrm(ks[11], (DEPTH, N_EXPERTS, 2 * D_FF), f32),
        "moe_w2": nrm(ks[12], (DEPTH, N_EXPERTS, D_FF, D_MODEL), f32) * (D_FF ** -0.5 * DN_BETA),
        "moe_b2": 0.01 * nrm(ks[13], (DEPTH, N_EXPERTS, D_MODEL), f32),
        "ple_w_proj": nrm(ks[14], (DEPTH, PLE_DIM, D_MODEL), f32) * PLE_DIM ** -0.5,
        "ple_w_gate": nrm(ks[15], (DEPTH, D_MODEL, D_MODEL), f32) * D_MODEL ** -0.5,
    }


def reference(x, p, ab_w_in, ab_w_out, c_w_in, c_w_out, ln_g, ln_b, router_w, router_b,
              moe_w1, moe_b1, moe_w2, moe_b2, ple_w_proj, ple_w_gate):
    for i in range(DEPTH):
        j = i // 2
        if i % 2 == 0:
            m = mixer_ab(x, ab_w_in[j], ab_w_out[j])
        else:
            m = mixer_c(x, c_w_in[j], c_w_out[j])
        x = layer_norm(DN_ALPHA * x + m, ln_g[i, 0], ln_b[i, 0])
        f = moe(x, router_w[i], router_b[i], moe_w1[i], moe_b1[i], moe_w2[i], moe_b2[i])
        x = layer_norm(DN_ALPHA * x + f, ln_g[i, 1], ln_b[i, 1])
        x = x + jax.nn.sigmoid(x @ ple_w_gate[i]) * (p[i] @ ple_w_proj[i])
    return x
```

```python
import numpy as np
import ml_dtypes
from contextlib import ExitStack
import concourse.bass as bass
import concourse.mybir as mybir
from concourse.bass_utils import run_bass_kernel_spmd

F32 = mybir.dt.float32
BF16 = mybir.dt.bfloat16
AF = mybir.ActivationFunctionType
ALU = mybir.AluOpType
AX = mybir.AxisListType
NPBF = ml_dtypes.bfloat16

NCORES = 8
S = 16384
D = 1024
ALPHA = 4.0 ** 0.25
EPS = 1e-5
NEG = -30000.0


class TL:
    __slots__ = ("t", "w", "r", "name", "excl")

    def __init__(self, t, name="", excl=False):
        self.t = t
        self.w = None
        self.r = {}
        self.name = name
        self.excl = excl

    def __getitem__(self, idx):
        return self.t[idx]


class Ker:
    NDMA = 24

    def __init__(self, nc):
        self.nc = nc
        self.es = ExitStack()
        self.sbes = ExitStack()
        self.eng = {"pe": nc.tensor, "dve": nc.vector, "act": nc.scalar, "pool": nc.gpsimd, "sp": nc.sync}
        self.sem = {}
        self.cnt = {}
        self.seen = {e: {} for e in self.eng}
        for e in ("pe", "dve", "act", "pool"):
            self.sem[e] = self.es.enter_context(nc.semaphore("s_" + e))
            self.cnt[e] = 0
        self.dsem = [self.es.enter_context(nc.semaphore("d%d" % i)) for i in range(self.NDMA)]
        self.dcnt = [0] * self.NDMA
        self.di = 0
        self.nins = 0
        self.out_tokens = []

    def sb(self, name, shape, dt):
        return TL(self.sbes.enter_context(self.nc.sbuf_tensor(name, shape, dt)), name)

    def ps(self, name, shape, dt=F32):
        return TL(self.sbes.enter_context(self.nc.psum_tensor(name, shape, dt)), name, excl=True)

    def _need(self, eng, tok, pend):
        if tok is None:
            return
        sem, val, key = tok
        if self.seen[eng].get(key, 0) >= val:
            return
        self.seen[eng][key] = val
        pend[key] = (sem, val)

    def _wait(self, eng, tok):
        pend = {}
        self._need(eng, tok, pend)
        for sem, val in pend.values():
            self.eng[eng].wait_ge(sem, val)
            self.nins += 1

    def _deps(self, eng, reads, writes, pend):
        for t in reads:
            if t.w is not None and not (eng == "pe" and t.w[2] == "pe"):
                self._need(eng, t.w, pend)
        for t in writes:
            if t.w is not None and not (eng == "pe" and t.w[2] == "pe"):
                self._need(eng, t.w, pend)
            for k, tok in t.r.items():
                if eng == "pe" and k == "pe":
                    continue
                self._need(eng, tok, pend)

    def _emit(self, eng, pend, fn):
        items = list(pend.values())
        for sem, val in items[:-1]:
            self.eng[eng].wait_ge(sem, val)
            self.nins += 1
        ins = fn(self.eng[eng])
        if items:
            ins._wait_ge(items[-1][0], items[-1][1])
        self.nins += 1
        return ins

    def _mark(self, tok, reads, writes):
        for t in reads:
            t.r[tok[2]] = tok
        for t in writes:
            t.w = tok
            t.r = {}

    def op(self, eng, fn, reads=(), writes=(), signal=True):
        ex = [t for t in reads if t.excl]
        if ex:
            reads = [t for t in reads if not t.excl]
            writes = list(writes) + ex
        pend = {}
        self._deps(eng, reads, writes, pend)
        ins = self._emit(eng, pend, fn)
        if signal:
            self.cnt[eng] += 1
            ins.then_inc(self.sem[eng], 1)
            tok = (self.sem[eng], self.cnt[eng], eng)
        else:
            tok = (self.sem[eng], self.cnt[eng] + 1, eng)
        self._mark(tok, reads, writes)
        return tok

    def dma(self, q, out, in_, reads=(), writes=(), **kw):
        i = self.di
        self.di = (self.di + 1) % self.NDMA
        key = "d%d" % i
        pend = {}
        if self.dcnt[i] > 0:
            self._need(q, (self.dsem[i], self.dcnt[i], key), pend)
        self._deps(q, reads, writes, pend)
        ins = self._emit(q, pend, lambda e: e.dma_start(out=out, in_=in_, **kw))
        ins.then_inc(self.dsem[i], 16)
        self.dcnt[i] += 16
        tok = (self.dsem[i], self.dcnt[i], key)
        self._mark(tok, reads, writes)
        return tok

    def new_phase(self, toks=()):
        for eng in ("pe", "dve", "act", "pool", "sp"):
            for e2 in ("pe", "dve", "act", "pool"):
                if self.cnt[e2] > 0:
                    self._wait(eng, (self.sem[e2], self.cnt[e2], e2))
            for tok in toks:
                self._wait(eng, tok)
        self.sbes.close()
        self.sbes = ExitStack()

    def finish(self, toks):
        for tok in toks:
            self._wait("sp", tok)


def _run(nc, in_maps):
    res = run_bass_kernel_spmd(nc, in_maps, core_ids=list(range(NCORES)))
    return res.results


TB = 1024
NTT = TB // 512
NH = 2


def build_ffn(with_proj1=False):
    nc = bass.Bass("TRN2", target_bir_lowering=False)
    K = Ker(nc)

    def din(name, shape, dt=F32):
        return nc.dram_tensor(name, shape, dt, kind="ExternalInput").ap()

    oT = din("oT", [D, NH * TB], BF16)
    xT = din("xT", [D, NH * TB])
    pT = din("pT", [256, NH * TB])
    w_out = din("w_out", [D, D])
    lnp = din("lnp", [128, 4, 8])
    w_r = din("w_r", [D, 32])
    b_r = din("b_r", [1, 32])
    import os
    NEXP = 32 if int(os.environ.get("FFN_STAGE", "99")) > 5 else 2
    w1 = din("w1", [NEXP, D, 2048])
    b1 = din("b1", [128, 32, 16])
    w2 = din("w2", [NEXP, D, D])
    b2 = din("b2", [32, D])
    w_g = din("w_g", [D, D])
    w_p = din("w_p", [256, D])
    ident_d = din("ident", [128, 128])
    yT = nc.dram_tensor("yT", [D, NH * TB], F32, kind="ExternalOutput").ap()

    acc = K.sb("acc", [128, 8, TB], F32)
    xbf = K.sb("xbf", [128, 8, TB], BF16)
    wA = [K.sb("wA%d" % i, [128, 8, 2048], BF16) for i in range(2)]
    wB = [K.sb("wB%d" % i, [128, 8, 1024], BF16) for i in range(2)]
    abuf = K.sb("abuf", [128, 8, 512], BF16)
    lnp_sb = K.sb("lnp_sb", [128, 4, 8], F32)
    b1_sb = K.sb("b1_sb", [128, 32, 16], F32)
    b1p_sb = K.sb("b1p_sb", [128, 32, 8], F32)
    b2_sb = K.sb("b2_sb", [32, D], F32)
    wr_sb = K.sb("wr_sb", [128, 8, 32], F32)
    br_sb = K.sb("br_sb", [1, 32], F32)
    ones_f = K.sb("ones_f", [128, 128], F32)
    ident = K.sb("ident_sb", [128, 128], F32)
    sel = K.sb("sel", [32, 32, 128], BF16)
    GT = K.sb("GT", [32, TB], F32)
    GTb = K.sb("GTb", [32, TB], BF16)
    gbc = [K.sb("gbc%d" % i, [128, 512], F32) for i in range(2)]
    stat = [K.sb("stat%d" % i, [128, 512], F32) for i in range(3)]
    tg = [K.sb("tg%d" % i, [128, 512], F32) for i in range(2)]
    sq = tg
    tsg = [K.sb("tsg%d" % i, [128, 512], F32) for i in range(2)]
    tu = [K.sb("tu%d" % i, [128, 512], F32) for i in range(2)]
    rt = K.sb("rt", [128, 96], F32)
    pbf = K.sb("pbf", [128, 2, TB], BF16)
    PS = [K.ps("ps%d" % i, [128, 512]) for i in range(8)]
    psi = [0]

    def nps():
        p = PS[psi[0] % 8]
        psi[0] += 1
        return p

    def wload(dst, src_ap, width):
        for c in range(8):
            K.dma("pool", dst[:, c, 0:width], src_ap[c * 128:(c + 1) * 128, :], writes=[dst])

    K.dma("sp", lnp_sb[:], lnp, writes=[lnp_sb])
    K.dma("sp", b1_sb[:], b1, writes=[b1_sb])
    K.dma("sp", b2_sb[:], b2, writes=[b2_sb])
    K.dma("sp", wr_sb[:], w_r.rearrange("(c p) e -> p c e", p=128), writes=[wr_sb])
    K.dma("sp", br_sb[:], b_r, writes=[br_sb])
    K.dma("sp", ident[:], ident_d, writes=[ident])
    ftoks = []
    for hf in range(NH):
        c0 = hf * TB
        K.dma("sp", acc[:], xT[:, c0:c0 + TB].rearrange("(c p) t -> p c t", p=128), writes=[acc])
        K.dma("sp", xbf[:], oT[:, c0:c0 + TB].rearrange("(c p) t -> p c t", p=128), writes=[xbf])
        STAGE = int(os.environ.get("FFN_STAGE", "99"))

        def done():
            tok = K.dma("sp", yT[:, c0:c0 + TB].rearrange("(c p) t -> p c t", p=128), acc[:], reads=[acc])
            K.finish([tok])
            print("ffn instructions:", K.nins, "stage", STAGE)
            return nc

        if STAGE == -2:
            return done()
        K.dma("pool", pbf[:], pT[:, c0:c0 + TB].rearrange("(c p) t -> p c t", p=128), writes=[pbf])
        wload(wB[0], w_out, 1024)
        if STAGE == -1:
            for t_ in (pbf, wB[0]):
                K._wait("sp", t_.w)
            return done()

        K.op("dve", lambda e: e.memset(ones_f[:], 1.0), writes=[ones_f])
        K.op("dve", lambda e: e.memset(sel[:], 0.0), writes=[sel])
        for ex in range(32):
            K.op("dve", lambda e, ex=ex: e.tensor_scalar(out=sel[:, ex, :], in0=sel[:, ex, :], scalar1=ident[0:32, ex:ex + 1],
                                                          scalar2=None, op0=ALU.add), reads=[ident, sel], writes=[sel])
        K.op("dve", lambda e: e.tensor_scalar(out=b1p_sb[:], in0=b1_sb[:, :, 8:16], scalar1=1.0, scalar2=None, op0=ALU.add),
             reads=[b1_sb], writes=[b1p_sb])

        if STAGE == 0:
            return done()
        def proj_residual(wt, src_bf, nk):
            for tt in range(NTT):
                ts = slice(tt * 512, (tt + 1) * 512)
                for f in range(8):
                    p = nps()
                    for k in range(nk):
                        K.op("pe", lambda e, p=p, k=k, f=f: e.matmul(p[:], lhsT=wt[:, k, f * 128:(f + 1) * 128], rhs=src_bf[:, k, ts],
                                                                      start=(k == 0), stop=(k == nk - 1)),
                             reads=[wt, src_bf], writes=[p], signal=(k == nk - 1))
                    yield tt, ts, f, p

        for tt, ts, f, p in proj_residual(wB[0], xbf, 8):
            K.op("dve", lambda e, p=p, f=f, ts=ts: e.scalar_tensor_tensor(out=acc[:, f, ts], in0=acc[:, f, ts], scalar=ALPHA, in1=p[:],
                                                                           op0=ALU.mult, op1=ALU.add), reads=[p, acc], writes=[acc])

        def layer_norm(li, write_bf=True):
            for tt in range(NTT):
                ts = slice(tt * 512, (tt + 1) * 512)
                p1 = nps()
                p2 = nps()
                for c in range(8):
                    K.op("pe", lambda e, c=c: e.matmul(p1[:], lhsT=ones_f[:], rhs=acc[:, c, ts], start=(c == 0), stop=(c == 7)),
                         reads=[ones_f, acc], writes=[p1], signal=(c == 7))
                for c in range(8):
                    s = sq[c % 2]
                    K.op("act", lambda e, c=c, s=s: e.activation(out=s[:], in_=acc[:, c, ts], func=AF.Square), reads=[acc], writes=[s])
                    K.op("pe", lambda e, c=c, s=s: e.matmul(p2[:], lhsT=ones_f[:], rhs=s[:], start=(c == 0), stop=(c == 7)),
                         reads=[ones_f, s], writes=[p2], signal=True)
                mean, rstd, t1 = stat[0], stat[1], stat[2]
                K.op("dve", lambda e: e.tensor_scalar(out=mean[:], in0=p1[:], scalar1=1.0 / D, scalar2=None, op0=ALU.mult), reads=[p1], writes=[mean])
                K.op("dve", lambda e: e.tensor_tensor(out=t1[:], in0=mean[:], in1=mean[:], op=ALU.mult), reads=[mean], writes=[t1])
                K.op("dve", lambda e: e.scalar_tensor_tensor(out=rstd[:], in0=p2[:], scalar=1.0 / D, in1=t1[:], op0=ALU.mult, op1=ALU.subtract),
                     reads=[p2, t1], writes=[rstd])
                K.op("dve", lambda e: e.tensor_scalar(out=rstd[:], in0=rstd[:], scalar1=EPS, scalar2=None, op0=ALU.add), reads=[rstd], writes=[rstd])
                K.op("act", lambda e: e.activation(out=rstd[:], in_=rstd[:], func=AF.Sqrt), reads=[rstd], writes=[rstd])
                K.op("dve", lambda e: e.reciprocal(out=rstd[:], in_=rstd[:]), reads=[rstd], writes=[rstd])
                for c in range(8):
                    K.op("dve", lambda e, c=c: e.tensor_tensor(out=acc[:, c, ts], in0=acc[:, c, ts], in1=mean[:], op=ALU.subtract),
                         reads=[acc, mean], writes=[acc])
                    K.op("dve", lambda e, c=c: e.tensor_tensor(out=acc[:, c, ts], in0=acc[:, c, ts], in1=rstd[:], op=ALU.mult),
                         reads=[acc, rstd], writes=[acc])
                    K.op("dve", lambda e, c=c: e.tensor_scalar(out=acc[:, c, ts], in0=acc[:, c, ts], scalar1=lnp_sb[:, 2 * li, c:c + 1],
                                                               scalar2=lnp_sb[:, 2 * li + 1, c:c + 1], op0=ALU.mult, op1=ALU.add),
                         reads=[acc, lnp_sb], writes=[acc])
                    if write_bf:
                        K.op("act", lambda e, c=c: e.copy(out=xbf[:, c, ts], in_=acc[:, c, ts]), reads=[acc], writes=[xbf])

        if STAGE == 1:
            return done()
        layer_norm(0)
        if STAGE == 2:
            return done()

        SUB = int(os.environ.get("FFN_SUB", "99"))
        for t128 in range(TB // 128):
            tk = slice(t128 * 128, (t128 + 1) * 128)
            p = nps()
            for c in range(8):
                K.op("pe", lambda e, c=c: e.matmul(p[:, 0:32], lhsT=acc[:, c, tk], rhs=wr_sb[:, c, :], start=(c == 0), stop=False),
                     reads=[acc, wr_sb], writes=[p], signal=False)
            K.op("pe", lambda e: e.matmul(p[:, 0:32], lhsT=ones_f[0:1, :], rhs=br_sb[:], start=False, stop=True),
                 reads=[ones_f, br_sb], writes=[p])
            lg, top8, ex_, gm = rt[:, 0:32], rt[:, 32:40], rt[:, 40:72], rt[:, 72:73]
            K.op("dve", lambda e: e.tensor_copy(out=lg, in_=p[:, 0:32]), reads=[p], writes=[rt])
            if SUB == 1:
                return done()
            K.op("dve", lambda e: e.max(out=top8, in_=lg), reads=[rt], writes=[rt])
            K.op("dve", lambda e: e.tensor_scalar(out=rt[:, 73:74], in0=rt[:, 32:33], scalar1=-1.0, scalar2=None, op0=ALU.mult), reads=[rt], writes=[rt])
            if SUB == 2:
                return done()
            K.op("act", lambda e: e.activation(out=ex_, in_=lg, func=AF.Exp, bias=rt[:, 73:74], scale=1.0), reads=[rt], writes=[rt])
            if SUB == 3:
                return done()
            K.op("dve", lambda e: e.scalar_tensor_tensor(out=ex_, in0=lg, scalar=rt[:, 35:36], in1=ex_, op0=ALU.is_ge, op1=ALU.mult),
                 reads=[rt], writes=[rt])
            if SUB == 4:
                return done()
            K.op("dve", lambda e: e.reduce_sum(out=gm, in_=ex_, axis=AX.X), reads=[rt], writes=[rt])
            K.op("dve", lambda e: e.reciprocal(out=gm, in_=gm), reads=[rt], writes=[rt])
            K.op("dve", lambda e: e.tensor_scalar(out=ex_, in0=ex_, scalar1=gm, scalar2=None, op0=ALU.mult), reads=[rt], writes=[rt])
            if SUB == 5:
                return done()
            pt = nps()
            K.op("pe", lambda e: e.matmul(pt[0:32, 0:128], lhsT=ex_, rhs=ident[:], start=True, stop=True), reads=[rt, ident], writes=[pt])
            K.op("act", lambda e, tk=tk: e.copy(out=GT[:, tk], in_=pt[0:32, 0:128]), reads=[pt], writes=[GT])
            K.op("dve", lambda e, tk=tk: e.tensor_copy(out=GTb[:, tk], in_=pt[0:32, 0:128]), reads=[pt], writes=[GTb])
            if SUB == 6:
                return done()

        if STAGE == 3:
            return done()
        for tt in range(NTT):
            ts = slice(tt * 512, (tt + 1) * 512)
            for f in range(8):
                p = nps()
                K.op("pe", lambda e, f=f: e.matmul(p[:], lhsT=b2_sb[:, f * 128:(f + 1) * 128], rhs=GT[:, ts], start=True, stop=True),
                     reads=[b2_sb, GT], writes=[p])
                K.op("dve", lambda e, p=p, f=f: e.scalar_tensor_tensor(out=acc[:, f, ts], in0=acc[:, f, ts], scalar=ALPHA, in1=p[:],
                                                                        op0=ALU.mult, op1=ALU.add), reads=[p, acc], writes=[acc])

        if STAGE == 4:
            return done()
        wload(wA[0], w1[0], 2048)
        wload(wB[1], w2[0], 1024)
        for ex in range(NEXP):
            A = wA[ex % 2]
            B = wB[(ex + 1) % 2]
            if ex + 1 < NEXP:
                wload(wA[(ex + 1) % 2], w1[ex + 1], 2048)
                wload(wB[ex % 2], w2[ex + 1], 1024)
            for tt in range(NTT):
                ts = slice(tt * 512, (tt + 1) * 512)
                g = gbc[tt % 2]
                pg = nps()
                K.op("pe", lambda e: e.matmul(pg[:], lhsT=sel[:, ex, :], rhs=GTb[:, ts], start=True, stop=True), reads=[sel, GTb], writes=[pg])
                K.op("act", lambda e: e.copy(out=g[:], in_=pg[:]), reads=[pg], writes=[g])
                for j in range(8):
                    ph = nps()
                    pu = nps()
                    for k in range(8):
                        K.op("pe", lambda e, k=k: e.matmul(ph[:], lhsT=A[:, k, j * 128:(j + 1) * 128], rhs=xbf[:, k, ts], start=(k == 0), stop=(k == 7)),
                             reads=[A, xbf], writes=[ph], signal=(k == 7))
                    for k in range(8):
                        K.op("pe", lambda e, k=k: e.matmul(pu[:], lhsT=A[:, k, 1024 + j * 128:1024 + (j + 1) * 128], rhs=xbf[:, k, ts], start=(k == 0), stop=(k == 7)),
                             reads=[A, xbf], writes=[pu], signal=(k == 7))
                    a, sg, u = tg[j % 2], tsg[j % 2], tu[j % 2]
                    K.op("dve", lambda e: e.tensor_scalar(out=a[:], in0=ph[:], scalar1=b1_sb[:, ex, j:j + 1], scalar2=7.0, op0=ALU.add, op1=ALU.min),
                         reads=[ph, b1_sb], writes=[a])
                    K.op("act", lambda e: e.activation(out=sg[:], in_=a[:], func=AF.Sigmoid, scale=1.702), reads=[a], writes=[sg])
                    K.op("dve", lambda e: e.tensor_scalar(out=u[:], in0=pu[:], scalar1=b1p_sb[:, ex, j:j + 1], scalar2=-6.0, op0=ALU.add, op1=ALU.max),
                         reads=[pu, b1p_sb], writes=[u])
                    K.op("dve", lambda e: e.scalar_tensor_tensor(out=u[:], in0=u[:], scalar=8.0, in1=a[:], op0=ALU.min, op1=ALU.mult),
                         reads=[u, a], writes=[u])
                    K.op("dve", lambda e: e.tensor_tensor(out=u[:], in0=u[:], in1=sg[:], op=ALU.mult), reads=[u, sg], writes=[u])
                    K.op("dve", lambda e: e.tensor_tensor(out=abuf[:, j, :], in0=u[:], in1=g[:], op=ALU.mult), reads=[u, g], writes=[abuf])
                for f in range(8):
                    py = nps()
                    for j in range(8):
                        K.op("pe", lambda e, j=j: e.matmul(py[:], lhsT=B[:, j, f * 128:(f + 1) * 128], rhs=abuf[:, j, :], start=(j == 0), stop=(j == 7)),
                             reads=[B, abuf], writes=[py], signal=(j == 7))
                    K.op("dve", lambda e, f=f: e.tensor_tensor(out=acc[:, f, ts], in0=acc[:, f, ts], in1=py[:], op=ALU.add), reads=[py, acc], writes=[acc])

        if STAGE == 5:
            return done()
        wload(wA[0], w_g, 1024)
        K.dma("pool", wB[0][:, 0:2, :], w_p.rearrange("(c p) f -> p c f", p=128), writes=[wB[0]])
        layer_norm(1)
        outs = []
        for tt in range(NTT):
            ts = slice(tt * 512, (tt + 1) * 512)
            for f in range(8):
                pgt = nps()
                pp = nps()
                for k in range(8):
                    K.op("pe", lambda e, k=k: e.matmul(pgt[:], lhsT=wA[0][:, k, f * 128:(f + 1) * 128], rhs=xbf[:, k, ts], start=(k == 0), stop=(k == 7)),
                         reads=[wA[0], xbf], writes=[pgt], signal=(k == 7))
                for k in range(2):
                    K.op("pe", lambda e, k=k: e.matmul(pp[:], lhsT=wB[0][:, k, f * 128:(f + 1) * 128], rhs=pbf[:, k, ts], start=(k == 0), stop=(k == 1)),
                         reads=[wB[0], pbf], writes=[pp], signal=(k == 1))
                sg = tsg[f % 2]
                K.op("act", lambda e: e.activation(out=sg[:], in_=pgt[:], func=AF.Sigmoid), reads=[pgt], writes=[sg])
                K.op("dve", lambda e: e.tensor_tensor(out=sg[:], in0=sg[:], in1=pp[:], op=ALU.mult), reads=[sg, pp], writes=[sg])
                K.op("dve", lambda e, f=f: e.tensor_tensor(out=acc[:, f, ts], in0=acc[:, f, ts], in1=sg[:], op=ALU.add), reads=[sg, acc], writes=[acc])
        ftoks.append(K.dma("sp", yT[:, c0:c0 + TB].rearrange("(c p) t -> p c t", p=128), acc[:], reads=[acc]))
    if with_proj1:
        K.new_phase(ftoks)
        K.finish(emit_proj1(nc, K, yT))
    else:
        K.finish(ftoks)
    print("ffn instructions:", K.nins)
    return nc

_CACHE = {}


def _prog(name, fn):
    if name not in _CACHE:
        _CACHE[name] = fn()
    return _CACHE[name]


def run_ffn(layer, oT_full, xT_full, inp, with_proj1=False):
    nc = _prog("ffn_p1" if with_proj1 else "ffn", lambda: build_ffn(with_proj1))
    if with_proj1:
        p1common, p1cs = proj1_host_inputs(inp)
        p1out = {nm: [] for nm in ("qT", "kT", "qiT", "kiT", "v", "wi")}
    i = layer
    w_out = inp["ab_w_out"][0] if i == 0 else inp["c_w_out"][0]
    lnp = np.ascontiguousarray(
        np.stack([inp["ln_g"][i, 0], inp["ln_b"][i, 0], inp["ln_g"][i, 1], inp["ln_b"][i, 1]], 0).reshape(4, 8, 128).transpose(2, 0, 1))
    import os
    NEXP = 32 if int(os.environ.get("FFN_STAGE", "99")) > 5 else 2
    w1 = inp["moe_w1"][i][:NEXP]
    w1d = np.ascontiguousarray(np.concatenate([w1[:, :, 0::2], w1[:, :, 1::2]], axis=2))
    b1 = inp["moe_b1"][i]
    b1d = np.concatenate([b1[:, 0::2], b1[:, 1::2]], axis=1)
    b1d = np.ascontiguousarray(b1d.reshape(32, 16, 128).transpose(2, 0, 1))
    common = {
        "w_out": np.ascontiguousarray(w_out), "lnp": lnp, "w_r": np.ascontiguousarray(inp["router_w"][i]),
        "b_r": np.ascontiguousarray(inp["router_b"][i][None, :]), "w1": w1d, "b1": b1d,
        "w2": np.ascontiguousarray(inp["moe_w2"][i][:NEXP]), "b2": np.ascontiguousarray(inp["moe_b2"][i]),
        "w_g": np.ascontiguousarray(inp["ple_w_gate"][i]), "w_p": np.ascontiguousarray(inp["ple_w_proj"][i]),
        "ident": np.eye(128, dtype=np.float32),
    }
    pT_full = np.ascontiguousarray(inp["p"][i, 0].T)
    out = np.empty((D, S), np.float32)
    TL_ = NH * TB
    for rnd in range(S // (NCORES * TL_)):
        maps = []
        for c in range(NCORES):
            t0 = (rnd * NCORES + c) * TL_
            m = dict(common)
            m["oT"] = np.ascontiguousarray(oT_full[:, t0:t0 + TL_])
            m["xT"] = np.ascontiguousarray(xT_full[:, t0:t0 + TL_])
            m["pT"] = np.ascontiguousarray(pT_full[:, t0:t0 + TL_])
            if with_proj1:
                m.update(p1common)
                m["cs"] = np.ascontiguousarray(p1cs[:, :, t0:t0 + TL_])
            maps.append(m)
        res = _run(nc, maps)
        for c in range(NCORES):
            t0 = (rnd * NCORES + c) * TL_
            out[:, t0:t0 + TL_] = res[c]["yT"]
            if with_proj1:
                for nm in p1out:
                    p1out[nm].append(res[c][nm])
    if with_proj1:
        pr = {nm: np.concatenate(p1out[nm], axis=(0 if nm in ("v", "wi") else 1)) for nm in p1out}
        pr["kiT"] = pr["kiT"][0:64]
        return out, pr
    return out


NQT = S // 512
NKB = S // 128


def build_attn0(nI=NQT):
    import os
    ASTAGE = int(os.environ.get("ATT_STAGE", "99"))
    nc = bass.Bass("TRN2", target_bir_lowering=False)
    K = Ker(nc)

    def din(name, shape, dt=F32):
        return nc.dram_tensor(name, shape, dt, kind="ExternalInput").ap()

    xT = din("xT", [D, S])
    wqk_d = din("wqk", [D, 6, 64])
    wv_d = din("wv", [D, 128])
    cs_d = din("cs", [64, 2, S])
    blkoh_d = din("blkoh", [64, S], BF16)
    SM_d = din("SM", [128, 4, 512], BF16)
    CM_d = din("CM", [128, 4, 512], BF16)
    U_d = din("U", [128, 128], BF16)
    idb_d = din("identb", [128, 128], BF16)
    oT = nc.dram_tensor("oT", [128, S], BF16, kind="ExternalOutput").ap()

    QB = K.sb("QB", [128, S], BF16)
    KB = K.sb("KB", [128, S], BF16)
    V = K.sb("V", [128, NKB, 64], BF16)
    QBt = [TL(QB.t) for _ in range(NQT)]
    KBt = [TL(KB.t) for _ in range(NQT)]
    SLt = [TL(QB.t) for _ in range(NQT)]
    Vt = [TL(V.t) for _ in range(NQT)]
    Wqk = K.sb("w_qk", [128, 8, 6, 64], BF16)
    Wv = K.sb("w_v", [128, 8, 128], BF16)
    SM = K.sb("SM_sb", [128, 4, 512], BF16)
    CM = K.sb("CM_sb", [128, 4, 512], BF16)
    U = K.sb("U_sb", [128, 128], BF16)
    ones_b = K.sb("ones_b", [128, 128], BF16)
    idb = K.sb("idb", [128, 128], BF16)
    xb = [K.sb("xb%d" % i, [128, 8, 512], BF16) for i in range(2)]
    cst = [K.sb("cst%d" % i, [64, 2, 512], F32) for i in range(2)]
    f32t = [K.sb("f32t%d" % i, [128, 512], F32) for i in range(14)]
    b16t = [K.sb("b16t%d" % i, [128, 512], BF16) for i in range(16)]
    kmb = K.sb("kmb", [64, 64], BF16)
    ksum = K.sb("ksum", [64, 64], F32)
    gs = [K.sb("gs%d" % i, [128, 80], F32) for i in range(2)]
    selb = [K.sb("selb%d" % i, [128, 128], BF16) for i in range(2)]
    ost = [K.sb("ost%d" % i, [64, 512], BF16) for i in range(2)]
    PS = [K.ps("ps%d" % i, [128, 512]) for i in range(5)]
    Oacc = K.ps("Oacc", [128, 512])
    Dacc = K.ps("Dacc", [128, 512])
    PSB = K.ps("psb", [128, 1024], BF16)
    ctr = {"ps": 0, "f": 0, "b": 0}

    def nps():
        ctr["ps"] += 1
        return PS[ctr["ps"] % 5]

    def nf():
        ctr["f"] += 1
        return f32t[ctr["f"] % 14]

    def nb():
        ctr["b"] += 1
        return b16t[ctr["b"] % 16]

    K.dma("pool", Wqk[:], wqk_d.rearrange("(c p) s f -> p c s f", p=128), writes=[Wqk])
    K.dma("pool", Wv[:], wv_d.rearrange("(c p) f -> p c f", p=128), writes=[Wv])
    K.dma("sp", SM[:], SM_d, writes=[SM])
    K.dma("sp", CM[:], CM_d, writes=[CM])
    K.dma("sp", U[:], U_d, writes=[U])
    K.dma("sp", idb[:], idb_d, writes=[idb])
    K.op("dve", lambda e: e.memset(ones_b[:], 1.0), writes=[ones_b])
    for sb_ in selb:
        K.op("dve", lambda e: e.memset(sb_[:], 0.0), writes=[sb_])
    xTv = xT.rearrange("(c p) t -> p c t", p=128)
    out_toks = []
    phases = [p_ for p_ in ("sb", "mb") if not (ASTAGE == 3 and p_ == "mb") and not (ASTAGE in (1, 2, 4) and p_ == "sb")]

    for phase in phases:
        mb = (phase == "mb")
        if mb:
            K.dma("sp", KB[64:128, :], blkoh_d, writes=KBt)
        else:
            K.op("dve", lambda e: e.memset(QB[64:128, :], 0.0), writes=QBt)
            K.op("dve", lambda e: e.memset(KB[64:128, :], 0.0), writes=KBt)
        for i in range(nI):
            ts = slice(i * 512, (i + 1) * 512)
            x_ = xb[i % 2]
            K.dma("pool", x_[:], xTv[:, :, ts], writes=[x_])
            wsel = (2, 3, 4, 5) if mb else (0, 1)
            pp = {}
            for n in wsel:
                p = nps()
                pp[n] = p
                for k in range(8):
                    K.op("pe", lambda e, k=k: e.matmul(p[0:64, :], lhsT=Wqk[:, k, n, :], rhs=x_[:, k, :], start=(k == 0), stop=(k == 7)),
                         reads=[Wqk, x_], writes=[p], signal=(k == 7))
            if not mb:
                K.op("act", lambda e: e.copy(out=QB[0:64, ts], in_=pp[0][0:64, :]), reads=[pp[0]], writes=[QBt[i]])
                K.op("act", lambda e: e.mul(out=KB[0:64, ts], in_=pp[1][0:64, :], mul=0.125), reads=[pp[1]], writes=[KBt[i]])
            else:
                c_ = cst[i % 2]
                K.dma("sp", c_[:], cs_d[:, :, ts], writes=[c_])
                for (a, ar, dst, dt_) in ((2, 3, QB, QBt[i]), (4, 5, KB, KBt[i])):
                    t1 = nf()
                    t2 = nf()
                    K.op("dve", lambda e: e.tensor_tensor(out=t1[0:64, :], in0=pp[a][0:64, :], in1=c_[:, 0, :], op=ALU.mult), reads=[pp[a], c_], writes=[t1])
                    K.op("dve", lambda e: e.tensor_tensor(out=t2[0:64, :], in0=pp[ar][0:64, :], in1=c_[:, 1, :], op=ALU.mult), reads=[pp[ar], c_], writes=[t2])
                    K.op("dve", lambda e: e.tensor_tensor(out=dst[0:64, ts], in0=t1[0:64, :], in1=t2[0:64, :], op=ALU.add), reads=[t1, t2], writes=[dt_])
            pv = nps()
            v0 = 64 if mb else 0
            for sub in range(4):
                for k in range(8):
                    K.op("pe", lambda e, k=k: e.matmul(pv[:, sub * 64:(sub + 1) * 64], lhsT=x_[:, k, sub * 128:(sub + 1) * 128], rhs=Wv[:, k, v0:v0 + 64],
                                                       start=(k == 0), stop=(k == 7)),
                         reads=[Wv, x_], writes=[pv], signal=(k == 7 and sub == 3))
            K.op("act", lambda e: e.copy(out=V[:, i * 4:(i + 1) * 4, :], in_=pv[:, 0:256].rearrange("p (s f) -> p s f", f=64)), reads=[pv], writes=[Vt[i]])

        if ASTAGE == 1:
            tok = K.dma("sp", oT[0:64, 0:nI * 512], QB[0:64, 0:nI * 512], reads=QBt[:nI])
            tok2 = K.dma("sp", oT[64:128, 0:nI * 512], KB[0:64, 0:nI * 512], reads=KBt[:nI])
            K.finish([tok, tok2])
            return nc

        if mb:
            nblk = nI * 2
            K.op("dve", lambda e: e.tensor_reduce(out=ksum[:, 0:nblk], in_=KB[0:64, 0:nI * 512].rearrange("p (n k) -> p n k", k=256),
                                                  axis=AX.X, op=ALU.add), reads=KBt[:nI], writes=[ksum])
            K.op("dve", lambda e: e.tensor_scalar(out=kmb[:, 0:nblk], in0=ksum[:, 0:nblk], scalar1=1.0 / 256, scalar2=None, op0=ALU.mult),
                 reads=[ksum], writes=[kmb])
            for qt in range(nI * 4):
                I = qt // 4
                cur = qt // 2
                pg = nps()
                K.op("pe", lambda e: e.matmul(pg[:, 0:nblk], lhsT=QB[0:64, qt * 128:(qt + 1) * 128], rhs=kmb[:, 0:nblk], start=True, stop=True),
                     reads=[QBt[I], kmb], writes=[pg])
                g = gs[qt % 2]
                sb_ = selb[qt % 2]
                K.op("dve", lambda e: e.memset(g[:, 0:72], -1e30), writes=[g])
                if cur > 0:
                    K.op("dve", lambda e: e.tensor_copy(out=g[:, 0:cur], in_=pg[:, 0:cur]), reads=[pg], writes=[g])
                K.op("dve", lambda e: e.max(out=g[:, 72:80], in_=g[:, 0:64]), reads=[g], writes=[g])
                K.op("dve", lambda e: e.tensor_scalar(out=g[:, 0:64], in0=g[:, 0:64], scalar1=g[:, 74:75], scalar2=None, op0=ALU.is_ge), reads=[g], writes=[g])
                K.op("dve", lambda e: e.tensor_scalar(out=sb_[:, 64:128], in0=g[:, 0:64], scalar1=-1.0, scalar2=-NEG, op0=ALU.add, op1=ALU.mult),
                     reads=[g], writes=[sb_])
                K.op("dve", lambda e: e.memset(sb_[:, 64 + cur:128], NEG), writes=[sb_])
                K.op("dve", lambda e: e.memset(sb_[:, 64 + cur:64 + cur + 1], 0.0), writes=[sb_])
                K.op("pe", lambda e: e.transpose(PSB[:, 0:128], sb_[:], idb[:]), reads=[sb_, idb], writes=[PSB])
                K.op("act", lambda e: e.copy(out=QB[64:128, qt * 128:(qt + 1) * 128], in_=PSB[64:128, 0:128]), reads=[PSB], writes=[SLt[I]])
            if ASTAGE == 2:
                tok = K.dma("sp", oT[0:64, 0:nI * 512], QB[64:128, 0:nI * 512], reads=SLt[:nI])
                K.finish([tok])
                return nc

        pairs = [(I, idx) for I in range(nI) for idx in range(4 * I + 4)]
        npairs = len(pairs)
        C = {}
        accs = (Oacc, Dacc)

        def epilogue_sb(I, acc):
            qs = slice(I * 512, (I + 1) * 512)
            o_ = ost[I % 2]
            K.op("act", lambda e: e.copy(out=o_[:], in_=acc[0:64, :]), reads=[acc], writes=[o_])
            out_toks.append(K.dma("sp", oT[0:64, qs], o_[:], reads=[o_]))

        def epilogue_mb(I):
            qs = slice(I * 512, (I + 1) * 512)
            o_ = ost[I % 2]
            rc = nf()
            K.op("dve", lambda e: e.reciprocal(out=rc[0:64, :], in_=Dacc[0:64, :]), reads=[Dacc], writes=[rc])
            K.op("dve", lambda e: e.tensor_tensor(out=o_[:], in0=Oacc[0:64, :], in1=rc[0:64, :], op=ALU.mult), reads=[Oacc, rc], writes=[o_])
            out_toks.append(K.dma("sp", oT[64:128, qs], o_[:], reads=[o_]))

        if not mb:
            Rst = {"R": None}
            for t in range(npairs + 3):
                if t < npairs:
                    I, idx = pairs[t]
                    nb_ = 4 * I + 4
                    bq = nb_ - 1 - idx
                    c = {"I": I, "idx": idx, "b": bq, "j": bq - 4 * I, "first": idx == 0, "last": idx == nb_ - 1,
                         "ks": slice(bq * 128, (bq + 1) * 128), "qs": slice(I * 512, (I + 1) * 512)}
                    C[t] = c
                    z = nps()
                    K.op("pe", lambda e: e.matmul(z[:], lhsT=KB[:, c["ks"]], rhs=QB[:, c["qs"]], start=True, stop=True),
                         reads=[KBt[bq // 4], QBt[I]], writes=[z])
                    c["z"] = z
                if 0 <= t - 3 < npairs:
                    c3 = C.pop(t - 3)
                    acc = accs[c3["I"] % 2]
                    K.op("pe", lambda e: e.matmul(acc[0:64, :], lhsT=V[:, c3["b"], :], rhs=c3["A"][:], start=c3["first"], stop=c3["last"]),
                         reads=[Vt[c3["b"] // 4], c3["A"]], writes=[acc])
                    if c3["last"]:
                        epilogue_sb(c3["I"], acc)
                if 0 <= t - 2 < npairs:
                    c2 = C[t - 2]
                    if c2["first"]:
                        Rst["R"] = None
                    Rp = Rst["R"]
                    c2["Rafter"] = Rp
                    if c2["b"] > 0:
                        Rn = nb()
                        if Rp is None:
                            K.op("dve", lambda e: e.tensor_copy(out=Rn[:], in_=c2["L"][:]), reads=[c2["L"]], writes=[Rn])
                        else:
                            K.op("dve", lambda e: e.tensor_tensor(out=Rn[:], in0=Rp[:], in1=c2["L"][:], op=ALU.add), reads=[Rp, c2["L"]], writes=[Rn])
                        Rst["R"] = Rn
                        c2["Rafter"] = Rn
                    T = nf()
                    K.op("dve", lambda e: e.tensor_tensor(out=T[:], in0=c2["sf"][:], in1=c2["SP"][:], op=ALU.subtract), reads=[c2["sf"], c2["SP"]], writes=[T])
                    c2["T"] = T
                if 0 <= t - 1 < npairs:
                    c1 = C[t - 1]
                    Rp = None if c1["first"] else C[t - 2]["Rafter"]
                    sf = nps()
                    K.op("pe", lambda e: e.matmul(sf[:], lhsT=U[:], rhs=c1["L"][:], start=True, stop=False), reads=[U, c1["L"]], writes=[sf], signal=False)
                    if Rp is not None:
                        K.op("pe", lambda e: e.matmul(sf[:], lhsT=ones_b[:], rhs=Rp[:], start=False, stop=False), reads=[ones_b, Rp], writes=[sf], signal=False)
                    K.op("pe", lambda e: e.matmul(sf[:], lhsT=KB[:, c1["ks"]], rhs=QB[:, c1["qs"]], start=False, stop=True),
                         reads=[KBt[c1["b"] // 4], QBt[c1["I"]]], writes=[sf])
                    c1["sf"] = sf
                if t < npairs:
                    c = C[t]
                    E = nf()
                    K.op("act", lambda e: e.activation(out=E[:], in_=c["z"][:], func=AF.Exp), reads=[c["z"]], writes=[E])
                    SP_ = nf()
                    K.op("act", lambda e: e.activation(out=SP_[:], in_=E[:], func=AF.Ln, bias=1.0, scale=1.0), reads=[E], writes=[SP_])
                    c["SP"] = SP_
                    c["E"] = E
                if 0 <= t - 2 < npairs:
                    c2 = C[t - 2]
                    A = nb()
                    if c2["j"] >= 0:
                        K.op("act", lambda e: e.activation(out=c2["E"][:], in_=c2["T"][:], func=AF.Exp), reads=[c2["T"]], writes=[c2["E"]])
                        K.op("dve", lambda e: e.tensor_tensor(out=A[:], in0=c2["E"][:], in1=SM[:, c2["j"], :], op=ALU.mult), reads=[c2["E"], SM], writes=[A])
                    else:
                        K.op("act", lambda e: e.activation(out=A[:], in_=c2["T"][:], func=AF.Exp), reads=[c2["T"]], writes=[A])
                    c2["A"] = A
                if t < npairs:
                    c = C[t]
                    L = nb()
                    if c["j"] >= 0:
                        K.op("dve", lambda e: e.scalar_tensor_tensor(out=L[:], in0=c["SP"][:], scalar=-1.0, in1=SM[:, c["j"], :], op0=ALU.mult, op1=ALU.mult),
                             reads=[c["SP"], SM], writes=[L])
                    else:
                        K.op("dve", lambda e: e.tensor_scalar(out=L[:], in0=c["SP"][:], scalar1=-1.0, scalar2=None, op0=ALU.mult), reads=[c["SP"]], writes=[L])
                    c["L"] = L
        else:
            for t in range(npairs + 2):
                if t < npairs:
                    I, idx = pairs[t]
                    nb_ = 4 * I + 4
                    bq = idx
                    c = {"I": I, "b": bq, "j": bq - 4 * I, "first": idx == 0, "last": idx == nb_ - 1}
                    C[t] = c
                    ks = slice(bq * 128, (bq + 1) * 128)
                    qs = slice(I * 512, (I + 1) * 512)
                    j = c["j"]
                    zm = nps()
                    K.op("pe", lambda e: e.matmul(zm[:], lhsT=KB[:, ks], rhs=QB[:, qs], start=True, stop=(j < 0)),
                         reads=[KBt[bq // 4], QBt[I], SLt[I]], writes=[zm], signal=(j < 0))
                    if j >= 0:
                        K.op("pe", lambda e: e.matmul(zm[:], lhsT=idb[:], rhs=CM[:, j, :], start=False, stop=True), reads=[idb, CM], writes=[zm])
                    c["zm"] = zm
                if 0 <= t - 2 < npairs:
                    c1 = C.pop(t - 2)
                    K.op("pe", lambda e: e.matmul(Oacc[0:64, :], lhsT=V[:, c1["b"], :], rhs=c1["Am"][:], start=c1["first"], stop=c1["last"]),
                         reads=[Vt[c1["b"] // 4], c1["Am"]], writes=[Oacc])
                    K.op("pe", lambda e: e.matmul(Dacc[0:64, :], lhsT=ones_b[:, 0:64], rhs=c1["Am"][:], start=c1["first"], stop=c1["last"]),
                         reads=[ones_b, c1["Am"]], writes=[Dacc])
                    if c1["last"]:
                        epilogue_mb(c1["I"])
                if t < npairs:
                    c = C[t]
                    Am = nb()
                    K.op("act", lambda e: e.activation(out=Am[:], in_=c["zm"][:], func=AF.Exp, scale=0.125), reads=[c["zm"]], writes=[Am])
                    c["Am"] = Am
    K.finish(out_toks)
    print("attn0 instructions:", K.nins)
    return nc


def attn0_consts():
    half = 32
    inv = (10000.0 ** (-np.arange(half, dtype=np.float32) / half)).astype(np.float32)
    ang = np.arange(S, dtype=np.float32)[None, :] * inv[:, None]
    cos = np.cos(ang).astype(np.float32)
    sin = np.sin(ang).astype(np.float32)
    cs = np.empty((64, 2, S), np.float32)
    cs[0:32, 0] = cos
    cs[32:64, 0] = cos
    cs[0:32, 1] = -sin
    cs[32:64, 1] = sin
    s_ = np.arange(128)[:, None, None]
    j_ = np.arange(4)[None, :, None]
    t_ = np.arange(512)[None, None, :]
    kpos = 128 * j_ + s_
    SM = (kpos < t_).astype(NPBF)
    CM = np.where(kpos > t_, NEG, 0.0).astype(NPBF)
    U = (np.arange(128)[:, None] > np.arange(128)[None, :]).astype(NPBF)
    blkoh = (np.arange(64)[:, None] == (np.arange(S)[None, :] // 256)).astype(NPBF)
    return {"cs": cs, "SM": np.ascontiguousarray(SM), "CM": np.ascontiguousarray(CM), "U": U, "blkoh": np.ascontiguousarray(blkoh),
            "identb": np.eye(128).astype(NPBF)}


def _swap_halves(w):
    return np.concatenate([w[:, 32:64], w[:, 0:32]], axis=1)


def attn0_maps(inp):
    xT = np.ascontiguousarray(inp["x"][0].T)
    w_in = inp["ab_w_in"][0]
    consts = attn0_consts()
    maps = []
    for c in range(NCORES):
        def col(part):
            return w_in[:, part * 512 + c * 64: part * 512 + (c + 1) * 64]
        qa, ka, va, qb, kb, vb = [col(p_) for p_ in range(6)]
        m = dict(consts)
        m["xT"] = xT
        m["wqk"] = np.ascontiguousarray(np.stack([qa, ka, qb, _swap_halves(qb), kb, _swap_halves(kb)], axis=1))
        m["wv"] = np.ascontiguousarray(np.concatenate([va, vb], 1))
        maps.append(m)
    return maps


def run_attn0(inp, nI=NQT):
    nc = _prog("attn0_%d" % nI, lambda: build_attn0(nI))
    res = _run(nc, attn0_maps(inp))
    oT_full = np.empty((D, S), NPBF)
    for c in range(NCORES):
        oT_full[c * 64:(c + 1) * 64] = res[c]["oT"][0:64]
        oT_full[512 + c * 64:512 + (c + 1) * 64] = res[c]["oT"][64:128]
    return oT_full


TC0 = S // NCORES


def build_proj1():
    nc = bass.Bass("TRN2", target_bir_lowering=False)
    K = Ker(nc)
    xT = nc.dram_tensor("xT", [D, TC0], F32, kind="ExternalInput").ap()
    K.finish(emit_proj1(nc, K, xT))
    print("proj1 instructions:", K.nins)
    return nc


def emit_proj1(nc, K, xT):
    def din(name, shape, dt=F32):
        return nc.dram_tensor(name, shape, dt, kind="ExternalInput").ap()

    def dout(name, shape, dt):
        return nc.dram_tensor(name, shape, dt, kind="ExternalOutput").ap()

    wd = {"q": din("wq", [D, 1024]), "qs": din("wqs", [D, 1024]), "k": din("wk", [D, 1024]), "ks": din("wks", [D, 1024]),
          "v": din("wv", [D, 1024]), "qi": din("wqi", [D, 512]), "qis": din("wqis", [D, 512]),
          "ki": din("wki", [D, 128]), "kis": din("wkis", [D, 128]), "wi": din("wwi", [D, 8])}
    widths = {"q": 1024, "qs": 1024, "k": 1024, "ks": 1024, "v": 1024, "qi": 512, "qis": 512, "ki": 128, "kis": 128, "wi": 8}
    cs_d = din("cs", [128, 2, TC0])
    qT_o = dout("qT", [1024, TC0], BF16)
    kT_o = dout("kT", [1024, TC0], BF16)
    v_o = dout("v", [TC0, 1024], BF16)
    qiT_o = dout("qiT", [512, TC0], BF16)
    kiT_o = dout("kiT", [128, TC0], BF16)
    wi_o = dout("wi", [TC0, 8], F32)

    W = {n: K.sb("p1w_" + n, [128, 8, widths[n]], BF16) for n in wd}
    for n in wd:
        K.dma("pool", W[n][:], wd[n].rearrange("(c p) f -> p c f", p=128), writes=[W[n]])
    xb = [K.sb("p1xb%d" % i, [128, 8, 512], BF16) for i in range(2)]
    cst = [K.sb("p1cst%d" % i, [128, 2, 512], F32) for i in range(2)]
    f32t = [K.sb("p1f32t%d" % i, [128, 512], F32) for i in range(4)]
    stg = [K.sb("p1stg%d" % i, [128, 512], BF16) for i in range(4)]
    vst = [K.sb("p1vst%d" % i, [128, 1024], BF16) for i in range(2)]
    wst = [K.sb("p1wst%d" % i, [128, 8], F32) for i in range(2)]
    PS = [K.ps("p1ps%d" % i, [128, 512]) for i in range(8)]
    ctr = {"ps": 0, "f": 0, "s": 0}

    def nps():
        ctr["ps"] += 1
        return PS[ctr["ps"] % 8]

    def nf():
        ctr["f"] += 1
        return f32t[ctr["f"] % 4]

    def nst():
        ctr["s"] += 1
        return stg[ctr["s"] % 4]

    xTv = xT.rearrange("(c p) t -> p c t", p=128)
    toks = []
    for i in range(TC0 // 512):
        ts = slice(i * 512, (i + 1) * 512)
        x_ = xb[i % 2]
        c_ = cst[i % 2]
        K.dma("pool", x_[:], xTv[:, :, ts], writes=[x_])
        K.dma("sp", c_[:], cs_d[:, :, ts], writes=[c_])
        for (a, ar, nch, out) in (("q", "qs", 8, qT_o), ("k", "ks", 8, kT_o), ("qi", "qis", 4, qiT_o), ("ki", "kis", 1, kiT_o)):
            for ch in range(nch):
                fs = slice(ch * 128, (ch + 1) * 128)
                p1 = nps()
                p2 = nps()
                for (p, wn) in ((p1, a), (p2, ar)):
                    for k in range(8):
                        K.op("pe", lambda e, k=k: e.matmul(p[:], lhsT=W[wn][:, k, fs], rhs=x_[:, k, :], start=(k == 0), stop=(k == 7)),
                             reads=[W[wn], x_], writes=[p], signal=(k == 7))
                t1 = nf()
                t2 = nf()
                o_ = nst()
                K.op("dve", lambda e: e.tensor_tensor(out=t1[:], in0=p1[:], in1=c_[:, 0, :], op=ALU.mult), reads=[p1, c_], writes=[t1])
                K.op("dve", lambda e: e.tensor_tensor(out=t2[:], in0=p2[:], in1=c_[:, 1, :], op=ALU.mult), reads=[p2, c_], writes=[t2])
                K.op("dve", lambda e: e.tensor_tensor(out=o_[:], in0=t1[:], in1=t2[:], op=ALU.add), reads=[t1, t2], writes=[o_])
                toks.append(K.dma("sp", out[fs, ts], o_[:], reads=[o_]))
        for sub in range(4):
            tk = slice(sub * 128, (sub + 1) * 128)
            vs_ = vst[sub % 2]
            for half in range(2):
                p = nps()
                for k in range(8):
                    K.op("pe", lambda e, k=k: e.matmul(p[:], lhsT=x_[:, k, tk], rhs=W["v"][:, k, half * 512:(half + 1) * 512], start=(k == 0), stop=(k == 7)),
                         reads=[W["v"], x_], writes=[p], signal=(k == 7))
                K.op("act", lambda e: e.copy(out=vs_[:, half * 512:(half + 1) * 512], in_=p[:]), reads=[p], writes=[vs_])
            r0 = i * 512 + sub * 128
            toks.append(K.dma("sp", v_o[r0:r0 + 128, :], vs_[:], reads=[vs_]))
            p = nps()
            for k in range(8):
                K.op("pe", lambda e, k=k: e.matmul(p[:, 0:8], lhsT=x_[:, k, tk], rhs=W["wi"][:, k, :], start=(k == 0), stop=(k == 7)),
                     reads=[W["wi"], x_], writes=[p], signal=(k == 7))
            ws_ = wst[sub % 2]
            K.op("act", lambda e: e.copy(out=ws_[:], in_=p[:, 0:8]), reads=[p], writes=[ws_])
            toks.append(K.dma("sp", wi_o[r0:r0 + 128, :], ws_[:], reads=[ws_]))
    return toks


def _swap_heads(w):
    n = w.shape[1] // 64
    w4 = w.reshape(w.shape[0], n, 2, 32)
    return np.ascontiguousarray(w4[:, :, ::-1, :].reshape(w.shape[0], n * 64))


def rope_table128():
    half = 32
    inv = (10000.0 ** (-np.arange(half, dtype=np.float32) / half)).astype(np.float32)
    ang = np.arange(S, dtype=np.float32)[None, :] * inv[:, None]
    cos = np.cos(ang).astype(np.float32)
    sin = np.sin(ang).astype(np.float32)
    cs = np.empty((128, 2, S), np.float32)
    for r in range(4):
        cs[r * 32:(r + 1) * 32, 0] = cos
        cs[r * 32:(r + 1) * 32, 1] = sin if (r % 2) else -sin
    return cs


def proj1_host_inputs(inp):
    w = inp["c_w_in"][0]
    wq, wk, wv, wqi, wki, wwi = w[:, 0:1024], w[:, 1024:2048], w[:, 2048:3072], w[:, 3072:3584], w[:, 3584:3648], w[:, 3648:3656]
    wki2 = np.concatenate([wki, wki], 1)
    common = {"wq": np.ascontiguousarray(wq), "wqs": _swap_heads(wq), "wk": np.ascontiguousarray(wk), "wks": _swap_heads(wk),
              "wv": np.ascontiguousarray(wv), "wqi": np.ascontiguousarray(wqi), "wqis": _swap_heads(wqi),
              "wki": np.ascontiguousarray(wki2), "wkis": _swap_heads(wki2), "wwi": np.ascontiguousarray(wwi)}
    return common, rope_table128()


def run_proj1(x1T, inp):
    nc = _prog("proj1", build_proj1)
    w = inp["c_w_in"][0]
    wq, wk, wv, wqi, wki, wwi = w[:, 0:1024], w[:, 1024:2048], w[:, 2048:3072], w[:, 3072:3584], w[:, 3584:3648], w[:, 3648:3656]
    wki2 = np.concatenate([wki, wki], 1)
    common = {"wq": np.ascontiguousarray(wq), "wqs": _swap_heads(wq), "wk": np.ascontiguousarray(wk), "wks": _swap_heads(wk),
              "wv": np.ascontiguousarray(wv), "wqi": np.ascontiguousarray(wqi), "wqis": _swap_heads(wqi),
              "wki": np.ascontiguousarray(wki2), "wkis": _swap_heads(wki2), "wwi": np.ascontiguousarray(wwi)}
    cs = rope_table128()
    maps = []
    for c in range(NCORES):
        m = dict(common)
        m["xT"] = np.ascontiguousarray(x1T[:, c * TC0:(c + 1) * TC0])
        m["cs"] = np.ascontiguousarray(cs[:, :, c * TC0:(c + 1) * TC0])
        maps.append(m)
    res = _run(nc, maps)
    out = {}
    for nm, ax in (("qT", 1), ("kT", 1), ("qiT", 1), ("kiT", 1), ("v", 0), ("wi", 0)):
        out[nm] = np.concatenate([res[c][nm] for c in range(NCORES)], axis=ax)
    out["kiT"] = out["kiT"][0:64]
    return out


NSLOT = 8
QPC = 2048
NBIS = 21


def dsa_slot_blocks(core):
    return [core, 15 - core, 16 + core, 31 - core, 32 + core, 47 - core, 48 + core, 63 - core]


def build_dsa(slots=tuple(range(NSLOT))):
    nc = bass.Bass("TRN2", target_bir_lowering=False)
    K = Ker(nc)
    U8 = mybir.dt.uint8

    def din(name, shape, dt=F32):
        return nc.dram_tensor(name, shape, dt, kind="ExternalInput").ap()

    q_d = din("q", [128, NSLOT, 8, 512], BF16)
    qi_d = din("qi", [64, NSLOT, 8, 256], BF16)
    wi_d = din("wi", [128, 16, 8])
    qpos_d = din("qpos", [128, 16])
    kT_d = din("kT", [16, 64, S], BF16)
    v_d = din("v", [8, 8, 128, 16, 2, 128], BF16)
    ki_d = din("ki", [64, S], BF16)
    iota_d = din("iota", [128, 512])
    idb_d = din("identb", [128, 128], BF16)
    idf_d = din("identf", [128, 128])
    oT = nc.dram_tensor("oT", [1024, QPC], BF16, kind="ExternalOutput").ap()

    sc = K.sb("sc", [128, S], F32)
    maskT2 = [K.sb("maskT%d" % i, [128, 128, 256], U8) for i in range(2)]
    junk = K.sb("junk", [128, 2048], BF16)
    q_sb2 = [K.sb("q_sb%d" % i, [128, 8, 512], BF16) for i in range(2)]
    qi_sb = K.sb("qi_sb", [64, 8, 256], BF16)
    kic = [K.sb("kic%d" % i, [64, 512], BF16) for i in range(2)]
    kc = [K.sb("kc%d" % i, [128, 2048], BF16) for i in range(2)]
    vc = [K.sb("vc%d" % i, [128, 16, 2, 128], BF16) for i in range(2)]
    idf = K.sb("idf", [128, 128], F32)
    ocp = [K.sb("ocp%d" % i, [128, 256], F32) for i in range(2)]
    rl = [K.sb("rl%d" % i, [128, 512], F32) for i in range(3)]
    eb = [K.sb("eb%d" % i, [128, 512], BF16) for i in range(5)]
    ab = [K.sb("ab%d" % i, [128, 512], BF16) for i in range(5)]
    mk = [K.sb("mk%d" % i, [128, 512], BF16) for i in range(2)]
    iota = K.sb("iota_sb", [128, 512], F32)
    idb = K.sb("idb", [128, 128], BF16)
    ones_b = K.sb("ones_b", [128, 64], BF16)
    wi_sb = K.sb("wi_sb", [128, 16, 8], F32)
    qpos = K.sb("qpos_sb", [128, 16], F32)
    sm = K.sb("sm", [128, 32], F32)
    ost = [K.sb("ost%d" % i, [64, 512], BF16) for i in range(2)]
    rcp = K.sb("rcp", [64, 512], F32)
    PS = [K.ps("ps%d" % i, [128, 512]) for i in range(5)]
    Oacc2 = [K.ps("Oacc%d" % i, [128, 512]) for i in range(2)]
    PSB = K.ps("psb", [128, 1024], BF16)
    ctr = {"ps": 0, "r": 0, "e": 0, "a": 0, "kic": 0, "kv": 0, "mk": 0, "o": 0}

    def rot(lst, key):
        ctr[key] += 1
        return lst[ctr[key] % len(lst)]

    K.dma("sp", iota[:], iota_d, writes=[iota])
    K.dma("sp", idb[:], idb_d, writes=[idb])
    K.dma("sp", idf[:], idf_d, writes=[idf])
    K.dma("sp", wi_sb[:], wi_d, writes=[wi_sb])
    K.dma("sp", qpos[:], qpos_d, writes=[qpos])
    K.op("dve", lambda e: e.memset(ones_b[:], 1.0), writes=[ones_b])

    LO, W_, MID, CNT, GE, HI, QREL, CN8 = (sm[:, 0:1], sm[:, 1:2], sm[:, 2:3], sm[:, 3:4], sm[:, 4:5], sm[:, 5:6], sm[:, 6:7], sm[:, 8:16])
    toks = []

    def index_units(k, si):
        nb = 16 * (k + 1)
        ext = nb * 128
        nch = ext // 512
        maskT = maskT2[si % 2]
        units = []

        def u_load():
            K.dma("sp", qi_sb[:], qi_d[:, k], writes=[qi_sb])
        units.append(u_load)
        for half in range(2):
            qt = 2 * k + half
            qcols = slice(half * 128, (half + 1) * 128)

            def u_score(ch, qt=qt, qcols=qcols):
                cs_ = slice(ch * 512, (ch + 1) * 512)
                kk = rot(kic, "kic")
                K.dma("sp", kk[:], ki_d[:, cs_], writes=[kk])
                for h in range(8):
                    pl = rot(PS, "ps")
                    K.op("pe", lambda e: e.matmul(pl[:], lhsT=qi_sb[:, h, qcols], rhs=kk[:], start=True, stop=True), reads=[qi_sb, kk], writes=[pl])
                    r = rot(rl, "r")
                    K.op("act", lambda e: e.activation(out=r[:], in_=pl[:], func=AF.Relu), reads=[pl], writes=[r])
                    if h == 0:
                        K.op("dve", lambda e: e.tensor_scalar(out=sc[:, cs_], in0=r[:], scalar1=wi_sb[:, qt, 0:1], scalar2=None, op0=ALU.mult),
                             reads=[r, wi_sb], writes=[sc])
                    else:
                        K.op("dve", lambda e: e.scalar_tensor_tensor(out=sc[:, cs_], in0=r[:], scalar=wi_sb[:, qt, h:h + 1], in1=sc[:, cs_],
                                                                     op0=ALU.mult, op1=ALU.add), reads=[r, wi_sb, sc], writes=[sc])
            for ch in range(nch):
                units.append(lambda ch=ch, f=u_score: f(ch))

            def u_bounds(qt=qt):
                K.op("dve", lambda e: e.tensor_reduce(out=LO, in_=sc[:, 0:ext], axis=AX.X, op=ALU.min), reads=[sc], writes=[sm])
                K.op("dve", lambda e: e.tensor_reduce(out=HI, in_=sc[:, 0:ext], axis=AX.X, op=ALU.max), reads=[sc], writes=[sm])
                for ch in range(4 * k, nch):
                    cs_ = slice(ch * 512, (ch + 1) * 512)
                    r = rot(rl, "r")
                    K.op("dve", lambda e: e.tensor_scalar(out=QREL, in0=qpos[:, qt:qt + 1], scalar1=float(-512 * ch), scalar2=None, op0=ALU.add),
                         reads=[qpos], writes=[sm])
                    K.op("dve", lambda e: e.tensor_scalar(out=r[:], in0=iota[:], scalar1=QREL, scalar2=-1e30, op0=ALU.is_gt, op1=ALU.mult),
                         reads=[iota, sm], writes=[r])
                    K.op("dve", lambda e: e.tensor_tensor(out=sc[:, cs_], in0=sc[:, cs_], in1=r[:], op=ALU.add), reads=[sc, r], writes=[sc])
                K.op("dve", lambda e: e.tensor_tensor(out=W_, in0=HI, in1=LO, op=ALU.subtract), reads=[sm], writes=[sm])
                K.op("dve", lambda e: e.tensor_scalar(out=W_, in0=W_, scalar1=0.5000001, scalar2=1e-6, op0=ALU.mult, op1=ALU.add), reads=[sm], writes=[sm])
                K.op("dve", lambda e: e.tensor_tensor(out=MID, in0=LO, in1=W_, op=ALU.add), reads=[sm], writes=[sm])
            units.append(u_bounds)
            npass = (ext + 2047) // 2048

            def u_bis():
                K.op("dve", lambda e: e.memset(CN8, 0.0), writes=[sm])
                for pz in range(npass):
                    K.op("dve", lambda e: e.tensor_scalar(out=junk[:], in0=sc[:, pz * 2048:(pz + 1) * 2048], scalar1=MID, scalar2=0.0,
                                                          op0=ALU.is_ge, op1=ALU.add, accum_out=sm[:, 8 + pz:9 + pz]), reads=[sc, sm], writes=[junk, sm])
                K.op("dve", lambda e: e.reduce_sum(out=CNT, in_=CN8, axis=AX.X), reads=[sm], writes=[sm])
                K.op("dve", lambda e: e.tensor_scalar(out=GE, in0=CNT, scalar1=255.5, scalar2=None, op0=ALU.is_ge), reads=[sm], writes=[sm])
                K.op("dve", lambda e: e.scalar_tensor_tensor(out=LO, in0=GE, scalar=W_, in1=LO, op0=ALU.mult, op1=ALU.add), reads=[sm], writes=[sm])
                K.op("dve", lambda e: e.tensor_scalar(out=W_, in0=W_, scalar1=0.5, scalar2=None, op0=ALU.mult), reads=[sm], writes=[sm])
                K.op("dve", lambda e: e.tensor_tensor(out=MID, in0=LO, in1=W_, op=ALU.add), reads=[sm], writes=[sm])
            for it in range(NBIS):
                units.append(u_bis)

            def u_mask(ch, qcols=qcols):
                cs_ = slice(ch * 512, (ch + 1) * 512)
                m_ = rot(mk, "mk")
                K.op("dve", lambda e: e.tensor_scalar(out=m_[:], in0=sc[:, cs_], scalar1=LO, scalar2=None, op0=ALU.is_ge), reads=[sc, sm], writes=[m_])
                off = (ch % 2) * 512
                for jb in range(4):
                    K.op("pe", lambda e: e.transpose(PSB[:, off + jb * 128:off + (jb + 1) * 128], m_[:, jb * 128:(jb + 1) * 128], idb[:]),
                         reads=[m_, idb], writes=[PSB], signal=(jb == 3))
                if ch % 2 == 1 or ch == nch - 1:
                    b0 = (ch // 2) * 8
                    nbk = 8 if ch % 2 == 1 else 4
                    K.op("act", lambda e: e.copy(out=maskT[:, b0:b0 + nbk, qcols], in_=PSB[:, 0:nbk * 128].rearrange("p (b t) -> p b t", t=128)),
                         reads=[PSB], writes=[maskT])
            for ch in range(nch):
                units.append(lambda ch=ch, f=u_mask: f(ch))
        return units

    def attn_units(k, si):
        nb = 16 * (k + 1)
        maskT = maskT2[si % 2]
        q_sb = q_sb2[si % 2]
        units = []

        def u_loadq():
            K.dma("sp", q_sb[:], q_d[:, k], writes=[q_sb])
        units.append(u_loadq)
        st = {"pend": []}

        def part1(c):
            for hh in range(2):
                K.op("pe", lambda e: e.matmul(Oacc2[hh][:, 0:256], lhsT=c["vv"][:, c["bl"], hh, :], rhs=c["A"][:, hh * 256:(hh + 1) * 256],
                                              start=c["first"], stop=c["last"]), reads=[c["vv"], c["A"]], writes=[Oacc2[hh]], signal=(hh == 1))

        def epi(hp):
            o_ = rot(ost, "o")
            for hh in range(2):
                cp = ocp[hh]
                K.op("act", lambda e: e.copy(out=cp[:], in_=Oacc2[hh][:, 0:256]), reads=[Oacc2[hh]], writes=[cp])
                pd = rot(PS, "ps")
                K.op("pe", lambda e: e.matmul(pd[0:64, 0:256], lhsT=idf[:, 64:128], rhs=cp[:], start=True, stop=True), reads=[idf, cp], writes=[pd])
                K.op("dve", lambda e: e.reciprocal(out=rcp[:, hh * 256:(hh + 1) * 256], in_=pd[0:64, 0:256]), reads=[pd], writes=[rcp])
                K.op("dve", lambda e: e.tensor_tensor(out=o_[:, hh * 256:(hh + 1) * 256], in0=cp[0:64, :], in1=rcp[:, hh * 256:(hh + 1) * 256], op=ALU.mult),
                     reads=[cp, rcp], writes=[o_])
            for hh in range(2):
                h = 2 * hp + hh
                toks.append(K.dma("sp", oT[h * 64:(h + 1) * 64, k * 256:(k + 1) * 256], o_[:, hh * 256:(hh + 1) * 256], reads=[o_]))

        def flush(keep):
            while len(st["pend"]) > keep:
                c = st["pend"].pop(0)
                part1(c)
                if c["last"]:
                    epi(c["hp"])

        for hp in range(8):
            for cc in range(nb // 16):
                def u_loadkv(hp=hp, cc=cc):
                    kk = rot(kc, "kv")
                    vv = vc[ctr["kv"] % 2]
                    K.dma("sp", kk[:], kT_d.rearrange("h d s -> (h d) s")[128 * hp:128 * (hp + 1), cc * 2048:(cc + 1) * 2048], writes=[kk])
                    K.dma("sp", vv[:], v_d[hp, cc], writes=[vv])
                    st["kk"], st["vv"] = kk, vv
                units.append(u_loadkv)
                for bl in range(16):
                    def u_pair(hp=hp, cc=cc, bl=bl):
                        kk, vv = st["kk"], st["vv"]
                        b = cc * 16 + bl
                        c = {"hp": hp, "bl": bl, "vv": vv, "first": b == 0, "last": b == nb - 1}
                        zz = rot(PS, "ps")
                        K.op("pe", lambda e: e.matmul(zz[:], lhsT=kk[:, bl * 128:(bl + 1) * 128], rhs=q_sb[:, hp, :], start=True, stop=True),
                             reads=[kk, q_sb], writes=[zz])
                        flush(2)
                        E = rot(eb, "e")
                        K.op("act", lambda e: e.activation(out=E[:], in_=zz[:], func=AF.Exp, scale=0.125), reads=[zz], writes=[E])
                        A = rot(ab, "a")
                        K.op("pool", lambda e: e.tensor_tensor(out=A[:, 0:256], in0=E[:, 0:256], in1=maskT[:, b, :], op=ALU.mult), reads=[E, maskT], writes=[A])
                        K.op("pool", lambda e: e.tensor_tensor(out=A[:, 256:512], in0=E[:, 256:512], in1=maskT[:, b, :], op=ALU.mult), reads=[E, maskT], writes=[A])
                        c["A"] = A
                        st["pend"].append(c)
                        if c["last"]:
                            flush(0)
                    units.append(u_pair)
        return units

    def merge(A, B):
        ia = ib = 0
        while ia < len(A) or ib < len(B):
            if ib >= len(B) or (ia < len(A) and ia * len(B) <= ib * len(A)):
                A[ia]()
                ia += 1
            else:
                B[ib]()
                ib += 1

    order = list(slots)
    for u in index_units(order[0], 0):
        u()
    for si, k in enumerate(order):
        A = attn_units(k, si)
        B = index_units(order[si + 1], si + 1) if si + 1 < len(order) else []
        merge(A, B)
    K.finish(toks)
    print("dsa instructions:", K.nins)
    return nc


DSA_GROUPS = ((0, 1, 2, 3, 4, 5, 6, 7),)


def run_dsa(pr, groups=DSA_GROUPS):
    qT, kT, v, qiT, kiT, wi = pr["qT"], pr["kT"], pr["v"], pr["qiT"], pr["kiT"], pr["wi"]
    kT3 = np.ascontiguousarray(kT.reshape(16, 64, S))
    v5 = v.reshape(8, 16, 128, 8, 2, 64).transpose(3, 0, 2, 1, 4, 5)
    v_re = np.ones((8, 8, 128, 16, 2, 128), NPBF)
    v_re[..., 0:64] = v5
    ki = np.ascontiguousarray(kiT)
    iota = np.ascontiguousarray(np.broadcast_to(np.arange(512, dtype=np.float32)[None, :], (128, 512)))
    identb = np.eye(128).astype(NPBF)
    maps = []
    for c in range(NCORES):
        blocks = dsa_slot_blocks(c)
        cols = np.concatenate([np.arange(I * 256, (I + 1) * 256) for I in blocks])
        q4 = qT[:, cols].reshape(8, 2, 64, NSLOT, 256)
        q_c = np.zeros((128, NSLOT, 8, 512), NPBF)
        q_c[0:64, :, :, 0:256] = q4[:, 0].transpose(1, 2, 0, 3)
        q_c[64:128, :, :, 256:512] = q4[:, 1].transpose(1, 2, 0, 3)
        qi_c = qiT[:, cols].reshape(8, 64, NSLOT, 256).transpose(1, 2, 0, 3)
        wi_c = wi[cols].reshape(16, 128, 8).transpose(1, 0, 2)
        qpos_c = cols.astype(np.float32).reshape(16, 128).T
        maps.append({"q": np.ascontiguousarray(q_c), "qi": np.ascontiguousarray(qi_c), "wi": np.ascontiguousarray(wi_c),
                     "qpos": np.ascontiguousarray(qpos_c), "kT": kT3, "v": v_re, "ki": ki, "iota": iota, "identb": identb,
                     "identf": np.eye(128, dtype=np.float32)})
    oT_full = np.zeros((D, S), NPBF)
    for grp in groups:
        nc = _prog("dsa_" + "_".join(map(str, grp)), lambda: build_dsa(tuple(grp)))
        res = _run(nc, maps)
        for c in range(NCORES):
            blocks = dsa_slot_blocks(c)
            for k in grp:
                I = blocks[k]
                oT_full[:, I * 256:(I + 1) * 256] = res[c]["oT"][:, k * 256:(k + 1) * 256]
    return oT_full


def kernel(**inputs):
    inp = {k: np.asarray(v) for k, v in inputs.items()}
    xT = np.ascontiguousarray(inp["x"][0].T)
    oT = run_attn0(inp)
    xT, pr = run_ffn(0, oT, xT, inp, with_proj1=True)
    oT = run_dsa(pr)
    xT = run_ffn(1, oT, xT, inp)
    return np.ascontiguousarray(xT.T)[None].astype(np.float32)
```
